# Optimizing a Trainium2 kernel written in Bass

```python
import math
import jax, jax.numpy as jnp
from jax import lax
import numpy as np

D_MODEL = 1024
BATCH = 4
SEQ = 4096
DEPTH = 4

HEAD_DIM = 64
REC_WIDTH = 3 * D_MODEL // 8
REC_HEADS = REC_WIDTH // HEAD_DIM
REC_CONV = 4
REC_C = 8.0
DIFF_WIDTH = 3 * D_MODEL // 8
DIFF_HEADS = DIFF_WIDTH // HEAD_DIM
DIFF_SUB = HEAD_DIM // 2
ROPE_THETA = 500000.0
ROPE_DIM = DIFF_SUB // 4
Q_BLOCK = 128
RET_WIDTH = D_MODEL // 4
RET_HEADS = RET_WIDTH // HEAD_DIM
RET_CHUNK = 128
RET_ROT_BASE = 10000.0
MIX_WIDTH = REC_WIDTH + DIFF_WIDTH + RET_WIDTH
IN_SIZES = (REC_WIDTH, REC_WIDTH,
            DIFF_WIDTH, DIFF_WIDTH, DIFF_WIDTH,
            RET_WIDTH, RET_WIDTH, RET_WIDTH, RET_WIDTH)
IN_WIDTH = sum(IN_SIZES)
D_FF = 2816
N_MOD = 9
EPS = 1e-6

kernel_name = "hybrid_macaron_rglru_diffattn_retention_adaln"


def rmsnorm(x, g):
    xf = x.astype(jnp.float32)
    y = xf * lax.rsqrt(jnp.mean(xf * xf, axis=-1, keepdims=True) + EPS)
    return (y * g.astype(jnp.float32)).astype(x.dtype)


def modulate(h, shift, scale):
    return h * (1.0 + scale) + shift


def swiglu(h, w_in, w_out):
    hg, hu = jnp.split(h @ w_in, 2, axis=-1)
    return (jax.nn.silu(hg) * hu) @ w_out


def rotary(x, pos, rot_dim, base):
    half = rot_dim // 2
    inv = jnp.power(jnp.float32(base), -jnp.arange(half, dtype=jnp.float32) * (2.0 / rot_dim))
    ang = pos.astype(jnp.float32)[:, :, None] * inv
    ang = ang.reshape(ang.shape[:2] + (1,) * (x.ndim - 3) + (half,))
    cos, sin = jnp.cos(ang), jnp.sin(ang)
    xr = x[..., :rot_dim].astype(jnp.float32)
    x1, x2 = xr[..., :half], xr[..., half:]
    rot = jnp.concatenate([x1 * cos - x2 * sin, x2 * cos + x1 * sin], axis=-1).astype(x.dtype)
    return jnp.concatenate([rot, x[..., rot_dim:]], axis=-1)


def _lru_combine(left, right):
    a_l, b_l = left
    a_r, b_r = right
    return a_l * a_r, a_r * b_l + b_r


def rglru_group(xr, gate, conv_w, conv_b, wa, ba, wx, bx, lam):
    B, T, _ = xr.shape
    xc = lax.conv_general_dilated(xr, conv_w[:, None, :], window_strides=(1,),
                                  padding=[(REC_CONV - 1, 0)],
                                  dimension_numbers=('NWC', 'WIO', 'NWC'),
                                  feature_group_count=REC_WIDTH) + conv_b
    xb = xc.reshape(B, T, REC_HEADS, HEAD_DIM)
    r = jax.nn.sigmoid(jnp.einsum('bthi,hij->bthj', xb, wa).reshape(B, T, REC_WIDTH) + ba)
    i = jax.nn.sigmoid(jnp.einsum('bthi,hij->bthj', xb, wx).reshape(B, T, REC_WIDTH) + bx)
    log_a = -REC_C * r.astype(jnp.float32) * jax.nn.softplus(-lam.astype(jnp.float32))
    a = jnp.exp(log_a)
    b = jnp.sqrt(-jnp.expm1(2.0 * log_a)) * (i * xc).astype(jnp.float32)
    _, h = lax.associative_scan(_lru_combine, (a, b), axis=1)
    return jax.nn.gelu(gate) * h.astype(gate.dtype)


def diff_attention_group(q, k, v, positions, lq1, lk1, lq2, lk2, subln_g, lambda_init):
    B, T, _ = q.shape
    q = rotary(q.reshape(B, T, DIFF_HEADS, 2, DIFF_SUB), positions, ROPE_DIM, ROPE_THETA)
    k = rotary(k.reshape(B, T, DIFF_HEADS, 2, DIFF_SUB), positions, ROPE_DIM, ROPE_THETA)
    q = q.transpose(0, 2, 3, 1, 4)
    k = k.transpose(0, 2, 3, 1, 4)
    v = v.reshape(B, T, DIFF_HEADS, HEAD_DIM).transpose(0, 2, 1, 3)
    scale = DIFF_SUB ** -0.5
    lam = (jnp.exp(jnp.sum(lq1.astype(jnp.float32) * lk1.astype(jnp.float32)))
           - jnp.exp(jnp.sum(lq2.astype(jnp.float32) * lk2.astype(jnp.float32))) + lambda_init)
    outs = []
    for blk in range(T // Q_BLOCK):
        s0 = blk * Q_BLOCK
        end = s0 + Q_BLOCK
        scores = jnp.einsum('bhsqd,bhskd->bhsqk', q[:, :, :, s0:end], k[:, :, :, :end]).astype(jnp.float32) * scale
        mask = jnp.arange(end)[None, :] <= (s0 + jnp.arange(Q_BLOCK))[:, None]
        p = jax.nn.softmax(jnp.where(mask, scores, -jnp.inf), axis=-1)
        w = p[:, :, 0] - lam * p[:, :, 1]
        outs.append(jnp.einsum('bhqk,bhkd->bhqd', w.astype(v.dtype), v[:, :, :end]))
    o = jnp.concatenate(outs, axis=2)
    o = rmsnorm(o, subln_g) * (1.0 - lambda_init)
    return o.transpose(0, 2, 1, 3).reshape(B, T, DIFF_WIDTH)


def retention_group(q, k, v, g, positions):
    B, T, _ = q.shape
    C = RET_CHUNK
    N = T // C
    q = rotary(q.reshape(B, T, RET_HEADS, HEAD_DIM), positions, HEAD_DIM, RET_ROT_BASE)
    k = rotary(k.reshape(B, T, RET_HEADS, HEAD_DIM), positions, HEAD_DIM, RET_ROT_BASE) * (HEAD_DIM ** -0.5)
    v = v.reshape(B, T, RET_HEADS, HEAD_DIM)
    to_chunks = lambda t: t.astype(jnp.float32).transpose(0, 2, 1, 3).reshape(B, RET_HEADS, N, C, HEAD_DIM)
    qc, kc, vc = to_chunks(q), to_chunks(k), to_chunks(v)
    lg = jnp.log1p(-jnp.exp2(-5.0 - jnp.arange(RET_HEADS, dtype=jnp.float32)))
    idx = jnp.arange(C, dtype=jnp.float32)
    rel = idx[:, None] - idx[None, :]
    decay = jnp.where(rel >= 0, jnp.exp(jnp.maximum(rel, 0.0)[None] * lg[:, None, None]), 0.0)
    intra = jnp.einsum('bhncd,bhnmd->bhncm', qc, kc) * decay[None, :, None]
    o_intra = jnp.einsum('bhncm,bhnme->bhnce', intra, vc)
    k_decay = jnp.exp((C - 1.0 - idx)[None, :] * lg[:, None])
    kv = jnp.einsum('bhncd,bhnce->bhnde', kc * k_decay[None, :, None, :, None], vc)
    chunk_decay = jnp.exp(C * lg)[None, :, None, None]

    def step(state, kv_i):
        return chunk_decay * state + kv_i, state

    _, r_prev = lax.scan(step, jnp.zeros((B, RET_HEADS, HEAD_DIM, HEAD_DIM), jnp.float32),
                         jnp.moveaxis(kv, 2, 0))
    r_prev = jnp.moveaxis(r_prev, 0, 2)
    q_decay = jnp.exp((idx + 1.0)[None, :] * lg[:, None])
    o_cross = jnp.einsum('bhncd,bhnde->bhnce', qc * q_decay[None, :, None, :, None], r_prev)
    o = (o_intra + o_cross).reshape(B, RET_HEADS, T, HEAD_DIM)
    o = o * lax.rsqrt(jnp.mean(o * o, axis=-1, keepdims=True) + EPS)
    o = o.transpose(0, 2, 1, 3).reshape(B, T, RET_WIDTH)
    return (jax.nn.silu(g.astype(jnp.float32)) * o).astype(g.dtype)


def setup_inputs(seed: int = 0) -> dict:
    key = jax.random.key(seed)
    ks = jax.random.split(key, 32)
    f32 = jnp.float32
    D, F, L = D_MODEL, D_FF, DEPTH
    nrm = lambda k, shape, s: jax.random.normal(k, shape, f32) * s
    x = nrm(ks[0], (BATCH, SEQ, D), 1.0)
    c = nrm(ks[1], (BATCH, D), 1.0)
    positions = (jnp.arange(SEQ, dtype=jnp.int32)[None, :]
                 + jax.random.randint(ks[2], (BATCH, 1), 0, 1024, dtype=jnp.int32))
    u = jax.random.uniform(ks[20], (L, REC_WIDTH), f32, 0.9, 0.999)
    p = u ** (1.0 / REC_C)
    rec_lambda = jnp.log(p) - jnp.log1p(-p)
    return {
        "x": x,
        "c": c,
        "positions": positions,
        "norm_ffn1_g": 1.0 + nrm(ks[3], (L, D), 0.02),
        "norm_mix_g": 1.0 + nrm(ks[4], (L, D), 0.02),
        "norm_ffn2_g": 1.0 + nrm(ks[5], (L, D), 0.02),
        "ada_w": nrm(ks[6], (L, D, N_MOD * D), 0.5 * D ** -0.5),
        "ada_b": nrm(ks[7], (L, N_MOD * D), 0.02),
        "ffn1_w_in": nrm(ks[8], (L, D, 2 * F), D ** -0.5),
        "ffn1_w_out": nrm(ks[9], (L, F, D), F ** -0.5),
        "ffn2_w_in": nrm(ks[10], (L, D, 2 * F), D ** -0.5),
        "ffn2_w_out": nrm(ks[11], (L, F, D), F ** -0.5),
        "w_in": nrm(ks[12], (L, D, IN_WIDTH), D ** -0.5),
        "w_out": nrm(ks[13], (L, MIX_WIDTH, D), MIX_WIDTH ** -0.5),
        "rec_conv_w": nrm(ks[14], (L, REC_CONV, REC_WIDTH), REC_CONV ** -0.5),
        "rec_conv_b": nrm(ks[15], (L, REC_WIDTH), 0.02),
        "rec_gate_a_w": nrm(ks[16], (L, REC_HEADS, HEAD_DIM, HEAD_DIM), HEAD_DIM ** -0.5),
        "rec_gate_a_b": nrm(ks[17], (L, REC_WIDTH), 0.02),
        "rec_gate_x_w": nrm(ks[18], (L, REC_HEADS, HEAD_DIM, HEAD_DIM), HEAD_DIM ** -0.5),
        "rec_gate_x_b": nrm(ks[19], (L, REC_WIDTH), 0.02),
        "rec_lambda": rec_lambda,
        "diff_lambda_q1": nrm(ks[21], (L, DIFF_SUB), 0.1),
        "diff_lambda_k1": nrm(ks[22], (L, DIFF_SUB), 0.1),
        "diff_lambda_q2": nrm(ks[23], (L, DIFF_SUB), 0.1),
        "diff_lambda_k2": nrm(ks[24], (L, DIFF_SUB), 0.1),
        "diff_subln_g": 1.0 + nrm(ks[25], (L, HEAD_DIM), 0.02),
        "final_norm_g": 1.0 + nrm(ks[26], (D,), 0.02),
    }


def reference(x, c, positions, norm_ffn1_g, norm_mix_g, norm_ffn2_g, ada_w, ada_b,
              ffn1_w_in, ffn1_w_out, ffn2_w_in, ffn2_w_out, w_in, w_out,
              rec_conv_w, rec_conv_b, rec_gate_a_w, rec_gate_a_b, rec_gate_x_w, rec_gate_x_b,
              rec_lambda, diff_lambda_q1, diff_lambda_k1, diff_lambda_q2, diff_lambda_k2,
              diff_subln_g, final_norm_g):
    split_at = [int(s) for s in np.cumsum(IN_SIZES)[:-1]]
    silu_c = jax.nn.silu(c)
    for l in range(DEPTH):
        mod = (silu_c @ ada_w[l] + ada_b[l])[:, None, :]
        sh1, sc1, g1, sh2, sc2, g2, sh3, sc3, g3 = jnp.split(mod, N_MOD, axis=-1)
        h = modulate(rmsnorm(x, norm_ffn1_g[l]), sh1, sc1)
        x = x + 0.5 * g1 * swiglu(h, ffn1_w_in[l], ffn1_w_out[l])
        h = modulate(rmsnorm(x, norm_mix_g[l]), sh2, sc2)
        rx, rg, dq, dk, dv, tq, tk, tv, tg = jnp.split(h @ w_in[l], split_at, axis=-1)
        y_rec = rglru_group(rx, rg, rec_conv_w[l], rec_conv_b[l], rec_gate_a_w[l], rec_gate_a_b[l],
                            rec_gate_x_w[l], rec_gate_x_b[l], rec_lambda[l])
        lambda_init = 0.8 - 0.6 * math.exp(-0.3 * l)
        y_diff = diff_attention_group(dq, dk, dv, positions, diff_lambda_q1[l], diff_lambda_k1[l],
                                      diff_lambda_q2[l], diff_lambda_k2[l], diff_subln_g[l], lambda_init)
        y_ret = retention_group(tq, tk, tv, tg, positions)
        y = jnp.concatenate([y_rec, y_diff, y_ret], axis=-1) @ w_out[l]
        x = x + g2 * y
        h = modulate(rmsnorm(x, norm_ffn2_g[l]), sh3, sc3)
        x = x + 0.5 * g3 * swiglu(h, ffn2_w_in[l], ffn2_w_out[l])
    return rmsnorm(x, final_norm_g)
```

```python
import math
import numpy as np
import concourse.bass as bass
import concourse.mybir as mybir
from concourse.bass_utils import run_bass_kernel_spmd

F32 = mybir.dt.float32
BF16 = mybir.dt.bfloat16
I32 = mybir.dt.int32
AF = mybir.ActivationFunctionType
ALU = mybir.AluOpType

P = 128
D = 1024
KD = 8
L = 4
T = 2048
T2 = 4096
TT = 512
NT = T // TT
F = 2816
FBW = 256
NFB = F // FBW
EPS = 1e-6


class Buf:
    __slots__ = ("name", "w", "rs", "dsem", "dcnt")

    def __init__(self, name):
        self.name = name
        self.w = None
        self.rs = {}
        self.dsem = None
        self.dcnt = 0


class Sched:
    SAME_ENGINE_SYNC = True

    def __init__(self, nc):
        self.nc = nc
        self.eng = {"pe": nc.tensor, "act": nc.scalar, "dve": nc.vector, "pool": nc.gpsimd, "sp": nc.sync}
        self.sem = {}
        self.cnt = {}
        for k in ("pe", "act", "dve", "pool"):
            self.sem[k] = nc.alloc_semaphore("sem_" + k)
            self.cnt[k] = 0
        self.seen = {k: {} for k in self.eng}
        self.nbuf = 0

    def buf(self, name=None):
        self.nbuf += 1
        return Buf(name or ("b%d" % self.nbuf))

    def bufs(self, *shape):
        if len(shape) == 1:
            return [self.buf() for _ in range(shape[0])]
        return [self.bufs(*shape[1:]) for _ in range(shape[0])]

    def _waits(self, eng, reads, writes):
        need = {}

        def add(rec):
            sem, val, key = rec
            if key == eng and (eng == "pe" or not self.SAME_ENGINE_SYNC):
                return
            cur = need.get(key)
            if cur is None or cur[1] < val:
                need[key] = (sem, val)

        for b in reads:
            if b.w is not None:
                add(b.w)
        for b in writes:
            if b.w is not None:
                add(b.w)
            for key, (sem, val) in b.rs.items():
                add((sem, val, key))
        e = self.eng[eng]
        seen = self.seen[eng]
        for key, (sem, val) in need.items():
            if seen.get(key, 0) < val:
                e.wait_ge(sem, val)
                seen[key] = val

    def op(self, eng, fn, reads=(), writes=()):
        self._waits(eng, reads, writes)
        inst = fn(self.eng[eng])
        self.cnt[eng] += 1
        inst.then_inc(self.sem[eng], 1)
        rec = (self.sem[eng], self.cnt[eng], eng)
        for b in reads:
            b.rs[eng] = (rec[0], rec[1])
        for b in writes:
            b.w = rec
            b.rs = {}
        return inst

    def dma(self, q, out, in_, reads=(), writes=(), own=None, **kw):
        self._waits(q, reads, writes)
        if own is None:
            own = (list(writes) + list(reads))[0]
        if own.dsem is None:
            own.dsem = self.nc.alloc_semaphore("d_" + own.name)
        inst = self.eng[q].dma_start(out=out, in_=in_, **kw)
        own.dcnt += 16
        inst.then_inc(own.dsem, 16)
        key = "d_" + own.name
        rec = (own.dsem, own.dcnt, key)
        for b in reads:
            b.rs[key] = (rec[0], rec[1])
        for b in writes:
            b.w = rec
            b.rs = {}
        return inst

    def finish(self, eng, bufs):
        self._waits(eng, (), bufs)


class Prog:
    def __init__(self, phases, layers):
        self.phases = phases
        self.layers = layers
        nc = bass.Bass("TRN2", target_bir_lowering=False)
        self.nc = nc
        self.S = Sched(nc)
        self.inputs = {}

    def din(self, name, shape, dt=F32):
        t = self.nc.dram_tensor(name, list(shape), dt, kind="ExternalInput").ap()
        self.inputs[name] = (tuple(shape), dt)
        return t

    def dout(self, name, shape, dt=F32):
        return self.nc.dram_tensor(name, list(shape), dt, kind="ExternalOutput").ap()

    def sb(self, name, shape, dt=F32):
        return self.nc.alloc_sbuf_tensor("sb_" + name, list(shape), dt)

    def ps(self, name, shape, dt=F32):
        return self.nc.alloc_psum_tensor(name, list(shape), dt)

    def alloc_common(self, with_x=True):
        S = self.S
        if with_x:
            self.xT = self.sb("xT", [P, KD, T], F32)
            self.xb = S.bufs(KD, NT)
            self.hT = self.sb("hT", [P, KD, T], BF16)
            self.hb = S.bufs(KD, NT)
        self.ones = self.sb("ones", [P, P], BF16)
        self.ones_b = S.buf("ones")
        self.co = self.sb("co", [P, L, 9, KD], F32)
        self.co_b = S.buf("co")
        self.banks = [self.ps("bank%d" % i, [P, 512], F32) for i in range(7)]
        self.bank_b = [S.buf("bank%d" % i) for i in range(7)]
        self.psbf = self.ps("bankbf", [P, 1024], BF16)
        self.psbf_b = S.buf("bankbf")
        S.op("dve", lambda e: e.memset(self.ones[:], 1.0), writes=[self.ones_b])
        self.eps_t = self.sb("eps", [P, 1], F32)
        self.eps_b = S.buf("eps")
        S.op("dve", lambda e: e.memset(self.eps_t[:], EPS), writes=[self.eps_b])
        self.one_t = self.sb("one", [P, 1], F32)
        S.op("dve", lambda e: e.memset(self.one_t[:], 1.0), writes=[self.eps_b])

    def alloc_ffn(self):
        S = self.S
        self.NWS = 2
        self.wi = [self.sb("wi%d" % i, [P, KD, 2, FBW], BF16) for i in range(self.NWS)]
        self.wi_b = [S.buf("wi%d" % i) for i in range(self.NWS)]
        self.wo = [self.sb("wo%d" % i, [P, 2, D], BF16) for i in range(self.NWS)]
        self.wo_b = [S.buf("wo%d" % i) for i in range(self.NWS)]
        self.aT = [self.sb("aT%d" % i, [P, 2, T], BF16) for i in range(2)]
        self.aT_b = S.bufs(2, 2, NT)
        self.st = [self.sb("st%d" % i, [P, TT], F32) for i in range(3)]
        self.st_b = S.bufs(3)
        self.st_i = 0
        self.sq = self.sb("sq", [P, KD, TT], BF16)
        self.sq_b = S.buf("sq")
        self.lnv = self.sb("lnv", [P, TT], F32)
        self.lnv_b = S.buf("lnv")
        self.wblk = 0

    def load_x(self, x_dram):
        S = self.S
        self.xld_b = S.buf("xld")
        for c in range(KD):
            S.dma("sp", self.xT[:, c, :], x_dram[:, c, :], writes=self.xb[c], own=self.xld_b)

    def store_x(self, x_dram):
        S = self.S
        self.xst_b = S.buf("xst")
        for c in range(KD):
            S.dma("sp", x_dram[:, c, :], self.xT[:, c, :], reads=self.xb[c], own=self.xst_b)
        S.finish("sp", [self.xst_b] + [b for r in self.xb for b in r])

    def load_co(self, co_dram):
        self.S.dma("sp", self.co[:], co_dram[:], writes=[self.co_b])

    def rmsnorm_mod(self, l, ia, ib, dst=None, dst_b=None, final_g=None):
        S = self.S
        dst = self.hT if dst is None else dst
        dst_b = self.hb if dst_b is None else dst_b
        bs, br = 6, 5
        for tt in range(NT):
            ts = slice(tt * TT, (tt + 1) * TT)
            S.op("act", lambda e: e.activation(out=self.sq[:], in_=self.xT[:, :, ts], func=AF.Square),
                 reads=[self.xb[c][tt] for c in range(KD)], writes=[self.sq_b])
            for c in range(KD):
                S.op("pe", lambda e: e.matmul(self.banks[bs][:], self.ones[:], self.sq[:, c, :],
                                              start=(c == 0), stop=(c == KD - 1)),
                     reads=[self.sq_b, self.ones_b], writes=[self.bank_b[bs]])
            S.op("act", lambda e: e.activation(out=self.lnv[:], in_=self.banks[bs][:], func=AF.Ln,
                                               scale=1.0 / D, bias=self.eps_t[:]),
                 reads=[self.bank_b[bs], self.eps_b], writes=[self.lnv_b])
            S.op("act", lambda e: e.activation(out=self.banks[br][:], in_=self.lnv[:], func=AF.Exp, scale=-0.5),
                 reads=[self.lnv_b], writes=[self.bank_b[br]])
            for c in range(KD):
                i = self.st_i % 3
                self.st_i += 1
                S.op("dve", lambda e: e.tensor_tensor(out=self.st[i][:], in0=self.xT[:, c, ts],
                                                      in1=self.banks[br][:], op=ALU.mult),
                     reads=[self.xb[c][tt], self.bank_b[br]], writes=[self.st_b[i]])
                if final_g is None:
                    sc_ap = self.co[:, l, ia, c:c + 1]
                    bi_ap = self.co[:, l, ib, c:c + 1]
                    S.op("act", lambda e: e.activation(out=dst[:, c, ts], in_=self.st[i][:], func=AF.Identity,
                                                       scale=sc_ap, bias=bi_ap),
                         reads=[self.st_b[i], self.co_b], writes=[dst_b[c][tt]])
                else:
                    sc_ap = final_g[:, c:c + 1]
                    S.op("act", lambda e: e.activation(out=dst[:, c, ts], in_=self.st[i][:], func=AF.Identity,
                                                       scale=sc_ap),
                         reads=[self.st_b[i], self.co_b, self.xb[c][tt]], writes=[dst_b[c][tt]])

    def ffn(self, l, which, win_d, wout_d, ig):
        S = self.S
        for fb in range(NFB):
            ws = self.wblk % self.NWS
            asl = self.wblk % 2
            self.wblk += 1
            S.dma("pool", self.wi[ws][:], win_d[l, which, fb], writes=[self.wi_b[ws]], max_dma_last_dim=4096)
            S.dma("pool", self.wo[ws][:], wout_d[l, which, fb], writes=[self.wo_b[ws]], max_dma_last_dim=4096)
            for fc in range(2):
                for tt in range(NT):
                    ts = slice(tt * TT, (tt + 1) * TT)
                    bg = (2 * fc + tt) % 2
                    bu = 2 + bg
                    for c in range(KD):
                        S.op("pe", lambda e: e.matmul(self.banks[bg][:], self.wi[ws][:, c, 0, fc * P:(fc + 1) * P],
                                                      self.hT[:, c, ts], start=(c == 0), stop=(c == KD - 1)),
                             reads=[self.wi_b[ws], self.hb[c][tt]], writes=[self.bank_b[bg]])
                    for c in range(KD):
                        S.op("pe", lambda e: e.matmul(self.banks[bu][:], self.wi[ws][:, c, 1, fc * P:(fc + 1) * P],
                                                      self.hT[:, c, ts], start=(c == 0), stop=(c == KD - 1)),
                             reads=[self.wi_b[ws], self.hb[c][tt]], writes=[self.bank_b[bu]])
                    i = self.st_i % 3
                    self.st_i += 1
                    S.op("act", lambda e: e.activation(out=self.st[i][:], in_=self.banks[bg][:], func=AF.Silu),
                         reads=[self.bank_b[bg]], writes=[self.st_b[i]])
                    S.op("dve", lambda e: e.tensor_tensor(out=self.aT[asl][:, fc, ts], in0=self.banks[bu][:],
                                                          in1=self.st[i][:], op=ALU.mult),
                         reads=[self.bank_b[bu], self.st_b[i]], writes=[self.aT_b[asl][fc][tt]])
            for dc in range(KD):
                for tt in range(NT):
                    ts = slice(tt * TT, (tt + 1) * TT)
                    by = 4 + (dc * NT + tt) % 2
                    for fc in range(2):
                        S.op("pe", lambda e: e.matmul(self.banks[by][:], self.wo[ws][:, fc, dc * P:(dc + 1) * P],
                                                      self.aT[asl][:, fc, ts], start=(fc == 0), stop=(fc == 1)),
                             reads=[self.wo_b[ws], self.aT_b[asl][fc][tt]], writes=[self.bank_b[by]])
                    g_ap = self.co[:, l, ig, dc:dc + 1]
                    S.op("dve", lambda e: e.scalar_tensor_tensor(out=self.xT[:, dc, ts], in0=self.banks[by][:],
                                                                 scalar=g_ap, in1=self.xT[:, dc, ts],
                                                                 op0=ALU.mult, op1=ALU.add),
                         reads=[self.bank_b[by], self.co_b, self.xb[dc][tt]], writes=[self.xb[dc][tt]])

    def ada(self, adaw_d, adab_d, cT_d, gains_d, layers):
        S = self.S
        ct = self.sb("ct", [P, KD], F32)
        sc2 = self.sb("sc2", [P, KD, 2], F32)
        adab = self.sb("adab", [P, L, 72], F32)
        gains = self.sb("gains", [P, L, 3, KD], F32)
        modT = self.sb("modT", [P, 72], F32)
        pieces = [self.sb("adap%d" % i, [P, KD, 512], F32) for i in range(2)]
        ct_b, sc2_b, adab_b, gains_b, mod_b = S.buf("ct"), S.buf("sc2"), S.buf("adab"), S.buf("gains"), S.buf("modT")
        pc_b = [S.buf("adap0"), S.buf("adap1")]
        S.dma("sp", ct[:], cT_d[:], writes=[ct_b])
        S.dma("sp", adab[:], adab_d[:], writes=[adab_b])
        S.dma("sp", gains[:], gains_d[:], writes=[gains_b])
        for j in range(2):
            S.op("act", lambda e: e.activation(out=sc2[:, :, j], in_=ct[:], func=AF.Silu), reads=[ct_b], writes=[sc2_b])
        bk = 6
        npc = 0
        for l in layers:
            src = adaw_d[l].rearrange("(kc p) n -> p kc n", p=P)
            for j in range(18):
                sl = npc % 2
                npc += 1
                S.dma("sp", pieces[sl][:], src[:, :, j * 512:(j + 1) * 512], writes=[pc_b[sl]])
                for q in range(4):
                    jn = j * 4 + q
                    for kc in range(KD):
                        S.op("pe", lambda e: e.matmul(self.banks[bk][:, 2 * jn:2 * jn + 2],
                                                      pieces[sl][:, kc, q * P:(q + 1) * P], sc2[:, kc, :],
                                                      start=(kc == 0), stop=(kc == KD - 1)),
                             reads=[pc_b[sl], sc2_b], writes=[self.bank_b[bk]])
            S.op("dve", lambda e: e.tensor_tensor(out=modT[:], in0=self.banks[bk][:, 0:144:2], in1=adab[:, l, :], op=ALU.add),
                 reads=[self.bank_b[bk], adab_b], writes=[mod_b])
            for i, (isc, ish, ig, gm) in enumerate([(8, 0, 16, 0.5), (32, 24, 40, 1.0), (56, 48, 64, 0.5)]):
                S.op("dve", lambda e: e.scalar_tensor_tensor(out=self.co[:, l, 3 * i, :], in0=modT[:, isc:isc + 8], scalar=1.0,
                                                             in1=gains[:, l, i, :], op0=ALU.add, op1=ALU.mult),
                     reads=[mod_b, gains_b], writes=[self.co_b])
                S.op("dve", lambda e: e.tensor_copy(out=self.co[:, l, 3 * i + 1, :], in_=modT[:, ish:ish + 8]),
                     reads=[mod_b], writes=[self.co_b])
                S.op("dve", lambda e: e.tensor_scalar(out=self.co[:, l, 3 * i + 2, :], in0=modT[:, ig:ig + 8], scalar1=gm,
                                                      scalar2=None, op0=ALU.mult),
                     reads=[mod_b], writes=[self.co_b])


def fm(a):
    n, d = a.shape
    return np.ascontiguousarray(a.reshape(n, d // P, P).transpose(2, 1, 0))


def fm_inv(a):
    p, c, n = a.shape
    return np.ascontiguousarray(a.transpose(2, 1, 0).reshape(n, c * p))


def vec_fm(v):
    sh = v.shape
    a = v.reshape(sh[:-1] + (sh[-1] // P, P))
    return np.ascontiguousarray(np.moveaxis(a, -1, 0))


def lay_ffn_in(w):
    a = w.reshape(L, KD, P, 2, NFB, FBW)
    return np.ascontiguousarray(a.transpose(0, 4, 2, 1, 3, 5))


def lay_ffn_out(w):
    a = w.reshape(L, NFB, 2, P, D)
    return np.ascontiguousarray(a.transpose(0, 1, 3, 2, 4))


NQ = T2 // TT
NF = 2112
O_RX, O_RG, O_DQ, O_DQS, O_DK, O_DKS, O_TQ, O_TQS, O_TK, O_TKS, O_DV, O_TV, O_TG = (
    0, 192, 384, 576, 768, 960, 1152, 1280, 1408, 1536, 1664, 1856, 1984)
MAGIC = 12582912.0
TWO_PI = 2.0 * math.pi
CW1 = 6.28125
CW2 = TWO_PI - CW1


class Mixer:
    def __init__(self, pr):
        self.pr = pr
        self.S = pr.S
        self.nc = pr.nc
        self.stop_stage = 99

    def alloc(self):
        pr, S = self.pr, self.S
        sb = pr.sb
        self.w = sb("mw", [P, KD, NF], BF16); self.w_b = S.buf("mw")
        self.hq = [sb("hq%d" % i, [P, KD, TT], BF16) for i in range(2)]; self.hq_b = S.bufs(2)
        self.tab = sb("tab", [P, 4, T2], BF16); self.tab_b = S.buf("tab")
        self.KdT = sb("KdT", [P, 2, T2], BF16); self.Kd_b = S.bufs(NQ)
        self.Va = sb("Va", [P, T2 // P, 3, 65], BF16); self.Va_b = S.bufs(NQ)
        self.cst = sb("mcst", [P, 12], F32); self.cst_b = S.buf("mcst")
        self.ident = sb("ident", [P, P], BF16); self.tri = sb("tri", [P, P], BF16)
        self.decT = sb("decT", [P, 2, P], F32); self.kdec = sb("kdec", [P, P], F32)
        self.qdecT = sb("qdecT", [P, TT], F32); self.gpow = sb("gpow", [P, 1], F32)
        self.cb = S.buf("mconst")
        self.recp = sb("recp", [P, 2, 8], F32); self.wab = sb("wab", [P, 2, 2, P], F32)
        self.lamv = sb("lamv", [P, 4, 32], F32); self.gsub = sb("gsub", [P, 64], F32); self.linit = sb("linit", [P, 2], F32)
        self.lp_b = S.buf("lparams")
        self.cL = sb("cL", [P, 2, 2], F32); self.cL_b = S.buf("cL")
        self.nlam = sb("nlam", [P, 1], F32); self.nlam_b = S.buf("nlam")
        self.gs2 = sb("gs2", [P, 64], F32); self.gs2_b = S.buf("gs2")
        self.rx = sb("rx", [P, 2, TT + 3], F32); self.rx_b = S.bufs(2)
        self.gel = sb("gel", [P, 2, TT], F32); self.gel_b = S.bufs(2)
        self.tmp = [sb("mt%d" % i, [P, TT], F32) for i in range(6)]; self.tmp_b = S.bufs(6); self.tmp_i = 0
        self.hs = [sb("hs%d" % i, [P, 2, TT], F32) for i in range(2)]; self.hs_b = S.bufs(2, 2)
        self.qd = sb("qd", [P, 2, TT], BF16); self.qd_b = S.bufs(2)
        self.qr = sb("qr", [P, TT], BF16); self.qr_b = S.buf("qr")
        self.kr = sb("kr", [P, TT], BF16); self.kr_b = S.buf("kr")
        self.qdec = sb("qdec", [P, TT], F32); self.qdec_b = S.buf("qdec")
        self.krm = [sb("krm%d" % i, [P, TT], BF16) for i in range(2)]; self.krm_b = S.bufs(2)
        self.qdm = [sb("qdm%d" % i, [P, TT], BF16) for i in range(2)]; self.qdm_b = S.bufs(2)
        self.qgm = [[sb("qgm%d_%d" % (j, g), [P, TT], BF16) for g in range(3)] for j in range(2)]; self.qgm_b = S.bufs(2, 3)
        self.vr = sb("vr", [P, 4, P], BF16); self.vr_b = S.bufs(4)
        self.sg = sb("sg", [P, 4, P], F32); self.sg_b = S.bufs(4)
        self.AT = [sb("AT%d" % i, [P, 2, P], BF16) for i in range(2)]; self.AT_b = S.bufs(2)
        self.kt = [sb("kt%d" % i, [P, P], BF16) for i in range(2)]; self.kt_b = S.bufs(2)
        self.St = sb("St", [P, 64], F32); self.St_b = S.buf("St")
        self.Sbf = [sb("Sbf%d" % i, [P, 64], BF16) for i in range(2)]; self.Sbf_b = S.bufs(2)
        self.PT = [sb("PT%d" % i, [P, TT], BF16) for i in range(3)]; self.PT_b = S.bufs(3); self.PT_i = 0
        self.t0 = sb("t0", [P, 4, 64], F32); self.t0_b = S.buf("t0")
        self.od = sb("od", [P, 4, 64], F32); self.od_b = S.buf("od")
        self.sml = sb("sml", [P, 16], F32); self.sml_b = S.buf("sml")
        self.ytok = sb("ytok", [P, 4, 384], BF16); self.ytok_b = S.bufs(4)
        self.oT = [sb("oT%d" % i, [P, 4, TT], BF16) for i in range(2)]; self.oT_b = S.bufs(2)
        self.bbf = pr.psbf
        self.bbf_b = pr.psbf_b

    def tmpbuf(self):
        i = self.tmp_i % len(self.tmp)
        self.tmp_i += 1
        return self.tmp[i], self.tmp_b[i]

    def setup(self, mconst_d, posrep_d):
        pr, S = self.pr, self.S
        for name in ("cst", "ident", "tri", "decT", "kdec", "qdecT", "gpow"):
            dst = getattr(self, name)
            S.dma("sp", dst[:], mconst_d[name][:], writes=[self.cb], own=self.cb)
        S.op("dve", lambda e: e.memset(self.Va[:, :, :, 64:65], 1.0), writes=[b for b in self.Va_b])
        S.op("dve", lambda e: e.memset(self.rx[:], 0.0), writes=self.rx_b)
        S.op("dve", lambda e: e.memset(self.St[:], 0.0), writes=[self.St_b])
        S.op("dve", lambda e: e.memset(self.Sbf[0][:], 0.0), writes=[self.Sbf_b[0]])
        S.op("dve", lambda e: e.memset(self.ytok[:], 0.0), writes=self.ytok_b)
        if getattr(self, 'skip_tables', False):
            return
        CW = 1024
        posi = pr.sb("posi", [P, CW], I32); posf = pr.sb("posf", [P, CW], F32)
        ang = pr.sb("ang", [P, CW], F32); kk = pr.sb("kk", [P, CW], F32)
        pb, fb_, ab, kb = S.buf("posi"), S.buf("posf"), S.buf("ang"), S.buf("kk")
        for cc in range(T2 // CW):
            csl = slice(cc * CW, (cc + 1) * CW)
            S.dma("sp", posi[:], posrep_d[:, csl], writes=[pb])
            S.op("dve", lambda e: e.tensor_copy(out=posf[:], in_=posi[:]), reads=[pb], writes=[fb_])
            for g in range(2):
                for cs in range(2):
                    fcol = self.cst[:, 2 * g:2 * g + 1]
                    S.op("dve", lambda e: e.tensor_scalar(out=ang[:], in0=posf[:], scalar1=fcol,
                                                          scalar2=(math.pi / 2 if cs == 0 else 0.0),
                                                          op0=ALU.mult, op1=ALU.add),
                         reads=[fb_, self.cb], writes=[ab])
                    S.op("dve", lambda e: e.tensor_scalar(out=kk[:], in0=ang[:], scalar1=1.0 / TWO_PI, scalar2=MAGIC,
                                                          op0=ALU.mult, op1=ALU.add), reads=[ab], writes=[kb])
                    S.op("dve", lambda e: e.tensor_scalar(out=kk[:], in0=kk[:], scalar1=MAGIC, scalar2=None,
                                                          op0=ALU.subtract), reads=[kb], writes=[kb])
                    S.op("dve", lambda e: e.scalar_tensor_tensor(out=ang[:], in0=kk[:], scalar=-CW1, in1=ang[:],
                                                                 op0=ALU.mult, op1=ALU.add), reads=[kb, ab], writes=[ab])
                    S.op("dve", lambda e: e.scalar_tensor_tensor(out=ang[:], in0=kk[:], scalar=-CW2, in1=ang[:],
                                                                 op0=ALU.mult, op1=ALU.add), reads=[kb, ab], writes=[ab])
                    S.op("dve", lambda e: e.tensor_scalar(out=ang[:], in0=ang[:], scalar1=-3.1415925, scalar2=3.1415925,
                                                          op0=ALU.max, op1=ALU.min), reads=[ab], writes=[ab])
                    if cs == 0:
                        S.op("act", lambda e: e.activation(out=self.tab[:, 2 * g, csl], in_=ang[:], func=AF.Sin),
                             reads=[ab], writes=[self.tab_b])
                    else:
                        S.op("act", lambda e: e.activation(out=self.tab[:, 2 * g + 1, csl], in_=ang[:], func=AF.Sin,
                                                           scale=self.cst[:, 2 * g + 1:2 * g + 2]),
                             reads=[ab, self.cb], writes=[self.tab_b])

    def layer_params(self, w_d, lp_d, lambda_init):
        S = self.S
        for c in range(KD):
            S.dma("pool", self.w[:, c, :], w_d[:, c, :], reads=[self.tab_b], writes=[self.w_b], own=self.w_b, max_dma_last_dim=4096)
        for name in ("recp", "wab", "lamv", "gsub", "linit"):
            S.dma("sp", getattr(self, name)[:], lp_d[name][:], writes=[self.lp_b], own=self.lp_b)
        sm = self.sml
        S.op("act", lambda e: e.activation(out=sm[:, 0:2], in_=self.recp[:, :, 7], func=AF.Exp, scale=-1.0),
             reads=[self.lp_b], writes=[self.sml_b])
        S.op("act", lambda e: e.activation(out=sm[:, 2:4], in_=sm[:, 0:2], func=AF.Ln, bias=1.0),
             reads=[self.sml_b], writes=[self.sml_b])
        S.op("dve", lambda e: e.tensor_scalar(out=self.cL[:, :, 0], in0=sm[:, 2:4], scalar1=-8.0, scalar2=None, op0=ALU.mult),
             reads=[self.sml_b], writes=[self.cL_b])
        S.op("dve", lambda e: e.tensor_scalar(out=self.cL[:, :, 1], in0=sm[:, 2:4], scalar1=-16.0, scalar2=None, op0=ALU.mult),
             reads=[self.sml_b], writes=[self.cL_b])
        pm, pm_b = self.tmpbuf()
        S.op("dve", lambda e: e.tensor_tensor(out=pm[:, 0:64], in0=self.lamv[:, 0:2, :], in1=self.lamv[:, 2:4, :], op=ALU.mult),
             reads=[self.lp_b], writes=[pm_b])
        S.op("dve", lambda e: e.tensor_reduce(out=sm[:, 4:6], in_=pm[:, 0:64].rearrange("p (a b) -> p a b", a=2),
                                              axis=mybir.AxisListType.X, op=ALU.add),
             reads=[pm_b], writes=[self.sml_b])
        S.op("act", lambda e: e.activation(out=sm[:, 6:8], in_=sm[:, 4:6], func=AF.Exp), reads=[self.sml_b], writes=[self.sml_b])
        S.op("dve", lambda e: e.tensor_tensor(out=sm[:, 8:9], in0=sm[:, 7:8], in1=sm[:, 6:7], op=ALU.subtract),
             reads=[self.sml_b], writes=[self.sml_b])
        S.op("dve", lambda e: e.tensor_tensor(out=self.nlam[:], in0=sm[:, 8:9], in1=self.linit[:, 0:1], op=ALU.add),
             reads=[self.sml_b, self.lp_b], writes=[self.nlam_b])
        S.op("dve", lambda e: e.tensor_scalar(out=self.gs2[:], in0=self.gsub[:], scalar1=self.linit[:, 1:2], scalar2=None, op0=ALU.mult),
             reads=[self.lp_b], writes=[self.gs2_b])

    def proj_fm(self, bank, hq, hq_b, col0, rows):
        pr, S = self.pr, self.S
        for c in range(KD):
            S.op("pe", lambda e: e.matmul(pr.banks[bank][0:rows, :], self.w[:, c, col0:col0 + rows], hq[:, c, :],
                                          start=(c == 0), stop=(c == KD - 1)),
                 reads=[self.w_b, hq_b], writes=[pr.bank_b[bank]])

    def rotary(self, b0, b1, rows, g, ts, out_ap, out_b):
        pr, S = self.pr, self.S
        t1, t1_b = self.tmpbuf()
        t2, t2_b = self.tmpbuf()
        S.op("dve", lambda e: e.tensor_tensor(out=t1[0:rows, :], in0=pr.banks[b0][0:rows, :], in1=self.tab[0:rows, 2 * g, ts], op=ALU.mult),
             reads=[pr.bank_b[b0], self.tab_b], writes=[t1_b])
        S.op("dve", lambda e: e.tensor_tensor(out=t2[0:rows, :], in0=pr.banks[b1][0:rows, :], in1=self.tab[0:rows, 2 * g + 1, ts], op=ALU.mult),
             reads=[pr.bank_b[b1], self.tab_b], writes=[t2_b])
        S.op("pool", lambda e: e.tensor_tensor(out=out_ap, in0=t1[0:rows, :], in1=t2[0:rows, :], op=ALU.add),
             reads=[t1_b, t2_b], writes=out_b)

    def tile(self, qt, h2T_d, oT_d):
        pr, S = self.pr, self.S
        banks, bank_b = pr.banks, pr.bank_b
        ts = slice(qt * TT, (qt + 1) * TT)
        sl = qt % 2
        hq, hq_b = self.hq[sl], self.hq_b[sl]
        S.dma("sp", hq[:], h2T_d[:, :, ts], writes=[hq_b])
        oT, oT_b = self.oT[sl], self.oT_b[sl]

        for m in range(4):
            ch = qt * 4 + m
            for c in range(KD):
                S.op("pe", lambda e: e.matmul(banks[2][:, 0:448], hq[:, c, m * P:(m + 1) * P], self.w[:, c, O_DV:O_DV + 448],
                                              start=(c == 0), stop=(c == KD - 1)),
                     reads=[self.w_b, hq_b], writes=[bank_b[2]])
            S.op("act", lambda e: e.activation(out=self.Va[:, ch, :, 0:64], in_=banks[2][:, 0:192].rearrange("p (h e) -> p h e", h=3),
                                               func=AF.Copy),
                 reads=[bank_b[2]], writes=[self.Va_b[qt]])
            S.op("act", lambda e: e.activation(out=self.vr[:, m, :], in_=banks[2][:, 192:320], func=AF.Copy),
                 reads=[bank_b[2]], writes=[self.vr_b[m]])
            S.op("act", lambda e: e.activation(out=self.sg[:, m, :], in_=banks[2][:, 320:448], func=AF.Silu),
                 reads=[bank_b[2]], writes=[self.sg_b[m]])

        if self.stop_stage <= 2:
            return
        for j, rows in ((0, 96), (1, 96)):
            self.proj_fm(0, hq, hq_b, O_DQ + j * 96, rows)
            self.proj_fm(1, hq, hq_b, O_DQS + j * 96, rows)
            self.rotary(0, 1, rows, 0, ts, self.qd[0:rows, j, :], [self.qd_b[j]])
            self.proj_fm(0, hq, hq_b, O_DK + j * 96, rows)
            self.proj_fm(1, hq, hq_b, O_DKS + j * 96, rows)
            self.rotary(0, 1, rows, 0, ts, self.KdT[0:rows, j, ts], [self.Kd_b[qt]])
        self.proj_fm(0, hq, hq_b, O_TQ, P)
        self.proj_fm(1, hq, hq_b, O_TQS, P)
        self.rotary(0, 1, P, 1, ts, self.qr[:], [self.qr_b])
        self.proj_fm(0, hq, hq_b, O_TK, P)
        self.proj_fm(1, hq, hq_b, O_TKS, P)
        self.rotary(0, 1, P, 1, ts, self.kr[:], [self.kr_b])
        S.op("dve", lambda e: e.tensor_tensor(out=self.qdec[:], in0=self.qr[:], in1=self.qdecT[:], op=ALU.mult),
             reads=[self.qr_b, self.cb], writes=[self.qdec_b])
        for h in range(2):
            S.op("dve", lambda e: e.tensor_scalar(out=self.krm[h][:], in0=self.kr[:], scalar1=self.cst[:, 4 + h:5 + h], scalar2=None, op0=ALU.mult),
                 reads=[self.kr_b, self.cb], writes=[self.krm_b[h]])
            S.op("dve", lambda e: e.tensor_scalar(out=self.qdm[h][:], in0=self.qdec[:], scalar1=self.cst[:, 4 + h:5 + h], scalar2=None, op0=ALU.mult),
                 reads=[self.qdec_b, self.cb], writes=[self.qdm_b[h]])
        for j in range(2):
            for g3 in range(3):
                S.op("dve", lambda e: e.tensor_scalar(out=self.qgm[j][g3][0:96, :], in0=self.qd[0:96, j, :], scalar1=self.cst[0:96, 6 + g3:7 + g3],
                                                      scalar2=None, op0=ALU.mult),
                     reads=[self.qd_b[j], self.cb], writes=[self.qgm_b[j][g3]])

        if self.stop_stage <= 3:
            return
        for j, rows in ((0, P), (1, 64)):
            R = slice(0, rows)
            self.proj_fm(0, hq, hq_b, O_RX + j * P, rows)
            S.op("act", lambda e: e.activation(out=self.rx[R, j, 3:3 + TT], in_=banks[0][R, :], func=AF.Copy),
                 reads=[bank_b[0]], writes=[self.rx_b[j]])
            self.proj_fm(1, hq, hq_b, O_RG + j * P, rows)
            t1, t1_b = self.tmpbuf()
            t2, t2_b = self.tmpbuf()
            S.op("act", lambda e: e.activation(out=t1[R, :], in_=banks[1][R, :], func=AF.Square), reads=[bank_b[1]], writes=[t1_b])
            S.op("dve", lambda e: e.tensor_scalar(out=t1[R, :], in0=t1[R, :], scalar1=0.044715, scalar2=1.0, op0=ALU.mult, op1=ALU.add),
                 reads=[t1_b], writes=[t1_b])
            S.op("dve", lambda e: e.tensor_tensor(out=t2[R, :], in0=banks[1][R, :], in1=t1[R, :], op=ALU.mult),
                 reads=[bank_b[1], t1_b], writes=[t2_b])
            S.op("act", lambda e: e.activation(out=t1[R, :], in_=t2[R, :], func=AF.Sigmoid, scale=1.5957691216057308),
                 reads=[t2_b], writes=[t1_b])
            S.op("dve", lambda e: e.tensor_tensor(out=self.gel[R, j, :], in0=banks[1][R, :], in1=t1[R, :], op=ALU.mult),
                 reads=[bank_b[1], t1_b], writes=[self.gel_b[j]])
            xc, xc_b = self.tmpbuf()
            S.op("act", lambda e: e.activation(out=xc[R, :], in_=self.rx[R, j, 3:3 + TT], func=AF.Identity,
                                               scale=self.recp[R, j, 3:4], bias=self.recp[R, j, 4:5]),
                 reads=[self.rx_b[j], self.lp_b], writes=[xc_b])
            for k in (1, 2, 3):
                S.op("dve", lambda e: e.scalar_tensor_tensor(out=xc[R, :], in0=self.rx[R, j, 3 - k:3 - k + TT],
                                                             scalar=self.recp[R, j, 3 - k:4 - k], in1=xc[R, :],
                                                             op0=ALU.mult, op1=ALU.add),
                     reads=[self.rx_b[j], self.lp_b, xc_b], writes=[xc_b])
            S.op("dve", lambda e: e.tensor_copy(out=self.rx[R, j, 0:3], in_=self.rx[R, j, TT:TT + 3]),
                 reads=[self.rx_b[j]], writes=[self.rx_b[j]])
            S.op("pe", lambda e: e.matmul(banks[0][R, :], self.wab[R, j, 0, 0:rows], xc[R, :], start=True, stop=True),
                 reads=[self.lp_b, xc_b], writes=[bank_b[0]])
            S.op("pe", lambda e: e.matmul(banks[1][R, :], self.wab[R, j, 1, 0:rows], xc[R, :], start=True, stop=True),
                 reads=[self.lp_b, xc_b], writes=[bank_b[1]])
            rr, rr_b = self.tmpbuf()
            ig, ig_b = self.tmpbuf()
            S.op("act", lambda e: e.activation(out=rr[R, :], in_=banks[0][R, :], func=AF.Sigmoid, bias=self.recp[R, j, 5:6]),
                 reads=[bank_b[0], self.lp_b], writes=[rr_b])
            S.op("act", lambda e: e.activation(out=ig[R, :], in_=banks[1][R, :], func=AF.Sigmoid, bias=self.recp[R, j, 6:7]),
                 reads=[bank_b[1], self.lp_b], writes=[ig_b])
            aa, aa_b = self.tmpbuf()
            S.op("act", lambda e: e.activation(out=aa[R, :], in_=rr[R, :], func=AF.Exp, scale=self.cL[R, j, 0:1]),
                 reads=[rr_b, self.cL_b], writes=[aa_b])
            S.op("act", lambda e: e.activation(out=rr[R, :], in_=rr[R, :], func=AF.Exp, scale=self.cL[R, j, 1:2]),
                 reads=[rr_b, self.cL_b], writes=[rr_b])
            S.op("act", lambda e: e.activation(out=rr[R, :], in_=rr[R, :], func=AF.Sqrt, scale=-1.0, bias=pr.one_t[R, :]),
                 reads=[rr_b, pr.eps_b], writes=[rr_b])
            S.op("pool", lambda e: e.tensor_tensor(out=ig[R, :], in0=ig[R, :], in1=xc[R, :], op=ALU.mult),
                 reads=[ig_b, xc_b], writes=[ig_b])
            S.op("dve", lambda e: e.tensor_tensor(out=ig[R, :], in0=ig[R, :], in1=rr[R, :], op=ALU.mult),
                 reads=[ig_b, rr_b], writes=[ig_b])
            hs, hs_b = self.hs[sl], self.hs_b[sl][j]
            if qt == 0:
                init = 0.0
                rd = []
            else:
                init = self.hs[1 - sl][R, j, TT - 1:TT]
                rd = [self.hs_b[1 - sl][j]]
            S.op("dve", lambda e: e.tensor_tensor_scan(out=hs[R, j, :], data0=aa[R, :], data1=ig[R, :], initial=init,
                                                       op0=ALU.mult, op1=ALU.add),
                 reads=[aa_b, ig_b] + rd, writes=[hs_b])
            dst = oT[R, 0, :] if j == 0 else oT[0:64, 3, :]
            S.op("dve", lambda e: e.tensor_tensor(out=dst, in0=hs[R, j, :], in1=self.gel[R, j, :], op=ALU.mult),
                 reads=[hs_b, self.gel_b[j]], writes=[oT_b])

        if self.stop_stage <= 4:
            return
        for m in range(4):
            n = qt * 4 + m
            cs = slice(m * P, (m + 1) * P)
            a_i = n % 2
            AT, AT_b = self.AT[a_i], self.AT_b[a_i]
            kt, kt_b = self.kt[a_i], self.kt_b[a_i]
            Sold, Sold_b = self.Sbf[n % 2], self.Sbf_b[n % 2]
            Snew, Snew_b = self.Sbf[(n + 1) % 2], self.Sbf_b[(n + 1) % 2]
            for h in range(2):
                hr = slice(64 * h, 64 * h + 64)
                S.op("pe", lambda e: e.matmul(banks[3][:, h * P:(h + 1) * P], self.krm[h][:, cs], self.qr[:, cs], start=True, stop=True),
                     reads=[self.krm_b[h], self.qr_b], writes=[bank_b[3]])
            S.op("dve", lambda e: e.tensor_tensor(out=AT[:], in0=banks[3][:, 0:2 * P].rearrange("p (h c) -> p h c", h=2),
                                                  in1=self.decT[:], op=ALU.mult),
                 reads=[bank_b[3], self.cb], writes=[AT_b])
            S.op("pe", lambda e: e.transpose(self.bbf[:, 0:P], self.kr[:, cs], self.ident[:]),
                 reads=[self.kr_b, self.cb], writes=[self.bbf_b])
            S.op("dve", lambda e: e.tensor_tensor(out=kt[:], in0=self.bbf[:, 0:P], in1=self.kdec[:], op=ALU.mult),
                 reads=[self.bbf_b, self.cb], writes=[kt_b])
            for h in range(2):
                hr = slice(64 * h, 64 * h + 64)
                S.op("pe", lambda e: e.matmul(banks[4][:, h * 64:(h + 1) * 64], AT[:, h, :], self.vr[:, m, hr], start=True, stop=False),
                     reads=[AT_b, self.vr_b[m]], writes=[bank_b[4]])
                S.op("pe", lambda e: e.matmul(banks[4][:, h * 64:(h + 1) * 64], self.qdm[h][:, cs], Sold[:, :], start=False, stop=True),
                     reads=[self.qdm_b[h], Sold_b], writes=[bank_b[4]])
            S.op("pe", lambda e: e.matmul(banks[3][:, 256:384], kt[:], self.vr[:, m, :], start=True, stop=True),
                 reads=[kt_b, self.vr_b[m]], writes=[bank_b[3]])
            for h in range(2):
                hr = slice(64 * h, 64 * h + 64)
                S.op("dve", lambda e: e.scalar_tensor_tensor(out=self.St[hr, :], in0=self.St[hr, :], scalar=self.gpow[hr, :],
                                                             in1=banks[3][hr, 256 + 64 * h:256 + 64 * h + 64],
                                                             op0=ALU.mult, op1=ALU.add),
                     reads=[self.St_b, self.cb, bank_b[3]], writes=[self.St_b])
            S.op("act", lambda e: e.activation(out=Snew[:], in_=self.St[:], func=AF.Copy), reads=[self.St_b], writes=[Snew_b])
            sm = self.sml
            for h in range(2):
                t1, t1_b = self.tmpbuf()
                S.op("act", lambda e: e.activation(out=t1[:, 0:64], in_=banks[4][:, h * 64:(h + 1) * 64], func=AF.Square,
                                                   accum_out=sm[:, 10 + h:11 + h]),
                     reads=[bank_b[4]], writes=[t1_b, self.sml_b])
            S.op("act", lambda e: e.activation(out=sm[:, 12:14], in_=sm[:, 10:12], func=AF.Ln, scale=1.0 / 64, bias=pr.eps_t[:]),
                 reads=[self.sml_b, pr.eps_b], writes=[self.sml_b])
            S.op("act", lambda e: e.activation(out=sm[:, 14:16], in_=sm[:, 12:14], func=AF.Exp, scale=-0.5),
                 reads=[self.sml_b], writes=[self.sml_b])
            for h in range(2):
                S.op("dve", lambda e: e.scalar_tensor_tensor(out=self.ytok[:, m, P + h * 64:P + (h + 1) * 64],
                                                             in0=banks[4][:, h * 64:(h + 1) * 64], scalar=sm[:, 14 + h:15 + h],
                                                             in1=self.sg[:, m, h * 64:(h + 1) * 64], op0=ALU.mult, op1=ALU.mult),
                     reads=[bank_b[4], self.sml_b, self.sg_b[m]], writes=[self.ytok_b[m]])

        if self.stop_stage <= 5:
            return
        scale = 32 ** -0.5
        for h in range(3):
            for s in range(2):
                g = 2 * h + s
                j = g // 3
                pb = 32 * (g % 3)
                pr_ = slice(pb, pb + 32)
                acc = 5
                nkc = 4 * qt + 4
                first = True
                for kc in range(nkc):
                    m = kc - 4 * qt
                    c0 = max(0, m) * P
                    sb_ = (6, 2)[kc % 2]
                    S.op("pe", lambda e: e.matmul(banks[sb_][:, c0:TT], self.KdT[0:96, j, kc * P:(kc + 1) * P], self.qgm[j][g % 3][0:96, c0:TT],
                                                  start=True, stop=True),
                         reads=[self.Kd_b[kc // 4], self.qgm_b[j][g % 3]], writes=[bank_b[sb_]])
                    pi = self.PT_i % 3
                    self.PT_i += 1
                    PT, PT_b = self.PT[pi], self.PT_b[pi]
                    S.op("act", lambda e: e.activation(out=PT[:, c0:TT], in_=banks[sb_][:, c0:TT], func=AF.Exp, scale=scale),
                         reads=[bank_b[sb_]], writes=[PT_b])
                    if m >= 0:
                        S.op("pool", lambda e: e.tensor_tensor(out=PT[:, c0:c0 + P], in0=PT[:, c0:c0 + P], in1=self.tri[:], op=ALU.mult),
                             reads=[PT_b, self.cb], writes=[PT_b])
                    for qb in range(max(0, m), 4):
                        S.op("pe", lambda e: e.matmul(banks[acc][:, qb * 65:qb * 65 + 65], PT[:, qb * P:(qb + 1) * P],
                                                      self.Va[:, kc, h, :], start=first, stop=(kc == 4 * qt + qb),
                                                      skip_group_check=True),
                             reads=[PT_b, self.Va_b[kc // 4]], writes=[bank_b[acc]])
                        first = False
                accv = banks[acc][:, 0:260].rearrange("p (q e) -> p q e", q=4)
                sm = self.sml
                S.op("dve", lambda e: e.reciprocal(out=sm[:, 0:4], in_=accv[:, :, 64]), reads=[bank_b[acc]], writes=[self.sml_b])
                if s == 0:
                    for qb in range(4):
                        S.op("dve", lambda e: e.tensor_scalar(out=self.t0[:, qb, :], in0=accv[:, qb, 0:64], scalar1=sm[:, qb:qb + 1],
                                                              scalar2=None, op0=ALU.mult),
                             reads=[bank_b[acc], self.sml_b], writes=[self.t0_b])
                else:
                    S.op("dve", lambda e: e.tensor_scalar(out=sm[:, 4:8], in0=sm[:, 0:4], scalar1=self.nlam[:, 0:1], scalar2=None, op0=ALU.mult),
                         reads=[self.sml_b, self.nlam_b], writes=[self.sml_b])
                    for qb in range(4):
                        S.op("dve", lambda e: e.scalar_tensor_tensor(out=self.od[:, qb, :], in0=accv[:, qb, 0:64], scalar=sm[:, 4 + qb:5 + qb],
                                                                     in1=self.t0[:, qb, :], op0=ALU.mult, op1=ALU.add),
                             reads=[bank_b[acc], self.sml_b, self.t0_b], writes=[self.od_b])
            sm = self.sml
            for qb in range(4):
                t1, t1_b = self.tmpbuf()
                S.op("act", lambda e: e.activation(out=t1[:, 0:64], in_=self.od[:, qb, :], func=AF.Square, accum_out=sm[:, 8 + qb:9 + qb]),
                     reads=[self.od_b], writes=[t1_b, self.sml_b])
            S.op("act", lambda e: e.activation(out=sm[:, 12:16], in_=sm[:, 8:12], func=AF.Ln, scale=1.0 / 64, bias=pr.eps_t[:]),
                 reads=[self.sml_b, pr.eps_b], writes=[self.sml_b])
            S.op("act", lambda e: e.activation(out=sm[:, 12:16], in_=sm[:, 12:16], func=AF.Exp, scale=-0.5),
                 reads=[self.sml_b], writes=[self.sml_b])
            for qb in range(4):
                col = h * 64 if h < 2 else 320
                S.op("dve", lambda e: e.scalar_tensor_tensor(out=self.ytok[:, qb, col:col + 64], in0=self.od[:, qb, :],
                                                             scalar=sm[:, 12 + qb:13 + qb], in1=self.gs2[:], op0=ALU.mult, op1=ALU.mult),
                     reads=[self.od_b, self.sml_b, self.gs2_b], writes=[self.ytok_b[qb]])

        if self.stop_stage <= 6:
            return
        for qb in range(4):
            cs = slice(qb * P, (qb + 1) * P)
            for blk in range(3):
                S.op("pe", lambda e: e.transpose(self.bbf[:, 128 + blk * P:128 + (blk + 1) * P], self.ytok[:, qb, blk * P:(blk + 1) * P], self.ident[:]),
                     reads=[self.ytok_b[qb], self.cb], writes=[self.bbf_b])
            S.op("act", lambda e: e.activation(out=oT[:, 1:3, cs], in_=self.bbf[:, 128:384].rearrange("p (b c) -> p b c", b=2), func=AF.Copy),
                 reads=[self.bbf_b], writes=[oT_b])
            S.op("act", lambda e: e.activation(out=oT[64:128, 3, cs], in_=self.bbf[64:128, 384:512], func=AF.Copy),
                 reads=[self.bbf_b], writes=[oT_b])
        S.dma("sp", oT_d[:, :, ts], oT[:], reads=[oT_b])


IN_OFF = {"rx": 0, "rg": 384, "dq": 768, "dk": 1152, "dv": 1536, "tq": 1920, "tk": 2176, "tv": 2432, "tg": 2688}


def _perm_diff():
    d = np.arange(192)
    r = d % 32
    return np.where(r < 4, d + 4, np.where(r < 8, d - 4, d))


def _perm_ret():
    d = np.arange(128)
    r = d % 64
    return np.where(r < 32, d + 32, d - 32)


def lay_mixer_w(w_in_l, s):
    def cols(name, width):
        o = IN_OFF[name] + width * s
        return w_in_l[:, o:o + width]
    dq, dk, tq, tk = cols("dq", 192), cols("dk", 192), cols("tq", 128), cols("tk", 128)
    pd, prr = _perm_diff(), _perm_ret()
    parts = [cols("rx", 192), cols("rg", 192), dq, dq[:, pd], dk, dk[:, pd], tq, tq[:, prr], tk, tk[:, prr],
             cols("dv", 192), cols("tv", 128), cols("tg", 128)]
    w = np.concatenate(parts, axis=1)
    assert w.shape[1] == NF
    return np.ascontiguousarray(w.reshape(KD, P, NF).transpose(1, 0, 2))


def mixer_consts(s):
    import ml_dtypes
    p = np.arange(P)
    cst = np.zeros((P, 12), np.float32)
    cst[:, 4] = (p < 64)
    cst[:, 5] = (p >= 64)
    for g_ in range(3):
        cst[:, 6 + g_] = (p // 32 == g_)
    r32 = p % 32
    invd = np.power(np.float32(500000.0), -np.arange(4, dtype=np.float32) * np.float32(2.0 / 8)).astype(np.float32)
    cst[:, 0] = np.where(r32 < 8, invd[r32 % 4], 0.0)
    cst[:, 1] = np.where(r32 < 4, -1.0, np.where(r32 < 8, 1.0, 0.0))
    r64 = p % 64
    invr = np.power(np.float32(10000.0), -np.arange(32, dtype=np.float32) * np.float32(2.0 / 64)).astype(np.float32)
    cst[:, 2] = invr[r64 % 32]
    cst[:, 3] = np.where(r64 < 32, -1.0, 1.0)
    ident = np.eye(P, dtype=np.float32).astype(ml_dtypes.bfloat16)
    tri = (p[:, None] <= p[None, :]).astype(np.float32).astype(ml_dtypes.bfloat16)
    lg = [math.log1p(-2.0 ** (-5.0 - (2 * s + h))) for h in range(2)]
    decT = np.zeros((P, 2, P), np.float64)
    kdec = np.zeros((P, P), np.float64)
    qdecT = np.zeros((P, TT), np.float64)
    gpow = np.zeros((P, 1), np.float64)
    rel = p[None, :] - p[:, None]
    for h in range(2):
        decT[:, h, :] = np.where(rel >= 0, 0.125 * np.exp(np.maximum(rel, 0) * lg[h]), 0.0)
        kdec[:, 64 * h:64 * h + 64] = (0.125 * np.exp((127.0 - p) * lg[h]))[:, None]
        qdecT[64 * h:64 * h + 64, :] = np.exp(((np.arange(TT) % P) + 1.0) * lg[h])[None, :]
        gpow[64 * h:64 * h + 64, 0] = math.exp(128.0 * lg[h])
    return {"cst": cst, "ident": ident, "tri": tri, "decT": decT.astype(np.float32), "kdec": kdec.astype(np.float32),
            "qdecT": qdecT.astype(np.float32), "gpow": gpow.astype(np.float32)}


MCONST_SHAPES = {"cst": ([P, 12], F32), "ident": ([P, P], BF16), "tri": ([P, P], BF16), "decT": ([P, 2, P], F32),
                 "kdec": ([P, P], F32), "qdecT": ([P, TT], F32), "gpow": ([P, 1], F32)}
LP_SHAPES = {"recp": [P, 2, 8], "wab": [P, 2, 2, P], "lamv": [P, 4, 32], "gsub": [P, 64], "linit": [P, 2]}


def mixer_layer_params(inp, l, s):
    recp = np.zeros((P, 2, 8), np.float32)
    wab = np.zeros((P, 2, 2, P), np.float32)
    ch0 = 192 * s
    vecs = [inp["rec_conv_w"][l][0], inp["rec_conv_w"][l][1], inp["rec_conv_w"][l][2], inp["rec_conv_w"][l][3],
            inp["rec_conv_b"][l], inp["rec_gate_a_b"][l], inp["rec_gate_x_b"][l], inp["rec_lambda"][l]]
    for k, v in enumerate(vecs):
        recp[:, 0, k] = v[ch0:ch0 + P]
        recp[0:64, 1, k] = v[ch0 + P:ch0 + 192]
    for a, name in enumerate(("rec_gate_a_w", "rec_gate_x_w")):
        g = inp[name][l]
        wab[0:64, 0, a, 0:64] = g[3 * s]
        wab[64:128, 0, a, 64:128] = g[3 * s + 1]
        wab[0:64, 1, a, 0:64] = g[3 * s + 2]
    lam = np.stack([inp["diff_lambda_q1"][l], inp["diff_lambda_q2"][l], inp["diff_lambda_k1"][l], inp["diff_lambda_k2"][l]], 0)
    lamv = np.ascontiguousarray(np.broadcast_to(lam[None], (P, 4, 32))).astype(np.float32)
    gsub = np.ascontiguousarray(np.broadcast_to(inp["diff_subln_g"][l][None], (P, 64))).astype(np.float32)
    li = lambda_init_of(l)
    linit = np.zeros((P, 2), np.float32)
    linit[:, 0] = -li
    linit[:, 1] = 1.0 - li
    return {"recp": recp, "wab": wab, "lamv": lamv, "gsub": gsub, "linit": linit}


def lambda_init_of(l):
    return 0.8 - 0.6 * math.exp(-0.3 * l)


def build_pa0():
    pr = Prog("A0", None)
    S = pr.S
    pr.alloc_common()
    pr.alloc_ffn()
    adaw = pr.din("ada_w", [L, D, 9 * D]); adab = pr.din("ada_b", [P, L, 72]); cT = pr.din("cT", [P, KD])
    gains = pr.din("gains", [P, L, 3, KD]); x0 = pr.din("x0", [P, KD, T])
    win = pr.din("win", [1, 1, NFB, P, KD, 2, FBW]); wout = pr.din("wout", [1, 1, NFB, P, 2, D])
    x1 = pr.dout("x1", [P, KD, T]); h2 = pr.dout("h2T", [P, KD, T], BF16); coo = pr.dout("coo", [P, L, 9, KD])
    pr.ada(adaw, adab, cT, gains, list(range(L)))
    S.dma("sp", coo[:], pr.co[:], reads=[pr.co_b])
    pr.load_x(x0)
    pr.rmsnorm_mod(0, 0, 1)
    pr.ffn(0, 0, win, wout, 2)
    pr.rmsnorm_mod(0, 3, 4)
    for c in range(KD):
        S.dma("sp", h2[:, c, :], pr.hT[:, c, :], reads=pr.hb[c])
    pr.store_x(x1)
    S.finish("sp", [pr.co_b] + [b for r in pr.hb for b in r])
    return pr


def build_pb():
    pr = Prog("B", None)
    S = pr.S
    pr.alloc_common(with_x=False)
    mx = Mixer(pr)
    mx.alloc()
    h2 = pr.din("h2T", [P, KD, T2], BF16); posrep = pr.din("posrep", [P, T2], I32); mw = pr.din("mw", [P, KD, NF])
    mc = {k: pr.din("mc_" + k, sh, dt) for k, (sh, dt) in MCONST_SHAPES.items()}
    lp = {k: pr.din("lp_" + k, sh) for k, sh in LP_SHAPES.items()}
    oT = pr.dout("oT", [P, 4, T2], BF16)
    mx.setup(mc, posrep)
    mx.layer_params(mw, lp, 0.0)
    for qt in range(NQ):
        mx.tile(qt, h2, oT)
    S.finish("sp", mx.oT_b)
    return pr


def build_pc(last):
    pr = Prog("C", None)
    S = pr.S
    pr.alloc_common()
    pr.alloc_ffn()
    nff = 1 if last else 2
    x_in = pr.din("x_in", [P, KD, T]); osel = pr.din("osel", [P, KD, T], BF16); co2 = pr.din("co2", [P, L, 9, KD])
    wop_d = pr.din("wop", [P, KD, D])
    win = pr.din("win", [1, nff, NFB, P, KD, 2, FBW]); wout = pr.din("wout", [1, nff, NFB, P, 2, D])
    wop = pr.sb("wop", [P, KD, D], BF16); wop_b = S.buf("wop")
    x_out = pr.dout("x_out", [P, KD, T])
    pr.load_co(co2)
    pr.load_x(x_in)
    for c in range(KD):
        S.dma("sp", pr.hT[:, c, :], osel[:, c, :], writes=pr.hb[c])
    for c in range(KD):
        S.dma("pool", wop[:, c, :], wop_d[:, c, :], writes=[wop_b], own=wop_b, max_dma_last_dim=4096)
    for dc in range(KD):
        for tt in range(NT):
            ts = slice(tt * TT, (tt + 1) * TT)
            by = 4 + (dc * NT + tt) % 2
            for kc in range(KD):
                S.op("pe", lambda e: e.matmul(pr.banks[by][:], wop[:, kc, dc * P:(dc + 1) * P], pr.hT[:, kc, ts],
                                              start=(kc == 0), stop=(kc == KD - 1)),
                     reads=[wop_b, pr.hb[kc][tt]], writes=[pr.bank_b[by]])
            S.op("dve", lambda e: e.scalar_tensor_tensor(out=pr.xT[:, dc, ts], in0=pr.banks[by][:], scalar=pr.co[:, 0, 5, dc:dc + 1],
                                                         in1=pr.xT[:, dc, ts], op0=ALU.mult, op1=ALU.add),
                 reads=[pr.bank_b[by], pr.co_b, pr.xb[dc][tt]], writes=[pr.xb[dc][tt]])
    pr.rmsnorm_mod(0, 6, 7)
    pr.ffn(0, 0, win, wout, 8)
    if not last:
        h2 = pr.dout("h2T", [P, KD, T], BF16)
        pr.rmsnorm_mod(1, 0, 1)
        pr.ffn(0, 1, win, wout, 2) if False else None
        pr_ffn_next(pr, win, wout)
        pr.rmsnorm_mod(1, 3, 4)
        for c in range(KD):
            S.dma("sp", h2[:, c, :], pr.hT[:, c, :], reads=pr.hb[c])
    else:
        fg_d = pr.din("fg", [P, KD])
        fg = pr.sb("fg", [P, KD], F32)
        S.dma("sp", fg[:], fg_d[:], writes=[pr.co_b], own=pr.co_b)
        pr.rmsnorm_mod(0, 0, 0, dst=pr.xT, dst_b=pr.xb, final_g=fg)
    pr.store_x(x_out)
    if not last:
        S.finish("sp", [b for r in pr.hb for b in r])
    return pr


def pr_ffn_next(pr, win, wout):
    orig_co = pr.co
    pr.co = _CoShift(orig_co)
    pr.ffn(0, 1, win, wout, 2)
    pr.co = orig_co


class _CoShift:
    def __init__(self, co):
        self.co = co

    def __getitem__(self, idx):
        p, l, i, c = idx
        return self.co[p, l + 1, i, c]


_CACHE = {}


def _run(pr, maps):
    res = run_bass_kernel_spmd(pr.nc, maps, core_ids=list(range(8)))
    return res.results


def _wop_rows(l_unused=None):
    rows = []
    for so in range(2):
        rec, dif, ret = 192 * so, 384 + 192 * so, 768 + 128 * so
        rows.append(np.arange(rec, rec + 128))
        rows.append(np.arange(dif, dif + 128))
        rows.append(np.arange(ret, ret + 128))
        rows.append(np.concatenate([np.arange(rec + 128, rec + 192), np.arange(dif + 128, dif + 192)]))
    return np.concatenate(rows)


def kernel(**inp):
    import ml_dtypes
    inp = {k: np.asarray(v) for k, v in inp.items()}
    B = inp["x"].shape[0]
    cores = [(r // 2, r % 2) for r in range(8)]
    win1, win2 = lay_ffn_in(inp["ffn1_w_in"]), lay_ffn_in(inp["ffn2_w_in"])
    wout1, wout2 = lay_ffn_out(inp["ffn1_w_out"]), lay_ffn_out(inp["ffn2_w_out"])
    gains_h = np.ascontiguousarray(np.stack([vec_fm(inp["norm_ffn1_g"]), vec_fm(inp["norm_mix_g"]), vec_fm(inp["norm_ffn2_g"])], 2))
    adab_h = vec_fm(inp["ada_b"])
    pa = build_pa0()
    maps = []
    for b, s in cores:
        maps.append({"ada_w": inp["ada_w"], "ada_b": adab_h, "cT": vec_fm(inp["c"][b]), "gains": gains_h,
                     "x0": fm(inp["x"][b, s * T:(s + 1) * T]), "win": win1[0][None, None], "wout": wout1[0][None, None]})
    res = _run(pa, maps)
    xs = [np.asarray(r["x1"]) for r in res]
    h2 = [np.asarray(r["h2T"]) for r in res]
    co = [np.asarray(r["coo"]) for r in res]
    pb = build_pb()
    pc_mid = build_pc(False)
    pc_last = build_pc(True)
    mconsts = [mixer_consts(s) for s in range(2)]
    rows = _wop_rows()
    for l in range(L):
        maps = []
        for r, (b, s) in enumerate(cores):
            m = {"h2T": np.ascontiguousarray(np.concatenate([h2[2 * b], h2[2 * b + 1]], axis=2)),
                 "posrep": np.ascontiguousarray(np.broadcast_to(inp["positions"][b][None], (P, T2))).astype(np.int32),
                 "mw": lay_mixer_w(inp["w_in"][l], s)}
            for k, v in mconsts[s].items():
                m["mc_" + k] = v
            for k, v in mixer_layer_params(inp, l, s).items():
                m["lp_" + k] = v
            maps.append(m)
        res = _run(pb, maps)
        oT = [np.asarray(r["oT"]) for r in res]
        last = (l == L - 1)
        wop_h = np.ascontiguousarray(inp["w_out"][l][rows].reshape(KD, P, D).transpose(1, 0, 2))
        maps = []
        for r, (b, s) in enumerate(cores):
            osel = np.ascontiguousarray(np.concatenate([oT[2 * b][:, :, s * T:(s + 1) * T], oT[2 * b + 1][:, :, s * T:(s + 1) * T]], axis=1))
            co2 = np.zeros((P, L, 9, KD), np.float32)
            co2[:, 0] = co[r][:, l]
            if not last:
                co2[:, 1] = co[r][:, l + 1]
                win = np.stack([win2[l], win1[l + 1]], 0)[None]
                wout = np.stack([wout2[l], wout1[l + 1]], 0)[None]
            else:
                win = win2[l][None, None]
                wout = wout2[l][None, None]
            m = {"x_in": xs[r], "osel": osel, "co2": co2, "wop": wop_h, "win": win, "wout": wout}
            if last:
                m["fg"] = vec_fm(inp["final_norm_g"])
            maps.append(m)
        res = _run(pc_last if last else pc_mid, maps)
        xs = [np.asarray(r["x_out"]) for r in res]
        if not last:
            h2 = [np.asarray(r["h2T"]) for r in res]
    out = np.zeros((B, 2 * T, D), np.float32)
    for r, (b, s) in enumerate(cores):
        out[b, s * T:(s + 1) * T] = fm_inv(xs[r])
    return out
```
